# Optimizing a Trainium2 kernel written in Bass

```python
import jax, jax.numpy as jnp
from jax import lax
import numpy as np

D_MODEL = 2048
BATCH = 2
SEQ = 16384
DEPTH = 2

N_A_LAYERS = DEPTH // 2
N_B_LAYERS = DEPTH - N_A_LAYERS
D_RNN = D_MODEL
N_RNN_BLOCKS = 8
RNN_BLOCK = D_RNN // N_RNN_BLOCKS
CONV_WIDTH = 4
LRU_C = 8.0
N_HEADS = 16
HEAD_DIM = D_MODEL // N_HEADS
Q_BLOCK = 128
FORGET_BIAS_MEAN = 4.0
N_GROUPS = 8
EXPERTS_PER_GROUP = 8
N_EXPERTS = N_GROUPS * EXPERTS_PER_GROUP
TOP_K = 2
D_EXPERT = 512
ROW_BLOCK = 128
EPS = 1e-6

kernel_name = 'hybrid_rglru_fox_hmoe'


def rms_norm(x, gain):
    x32 = x.astype(jnp.float32)
    y = x32 * lax.rsqrt(jnp.mean(x32 * x32, axis=-1, keepdims=True) + EPS)
    return (y * gain.astype(jnp.float32)).astype(x.dtype)


def rglru_mixer(h, w_in, conv_w, conv_b, w_rec, b_rec, w_inp, b_inp, lam, w_out):
    B, S, _ = h.shape
    proj = h @ w_in
    u, y = proj[..., :D_RNN], proj[..., D_RNN:]
    u = lax.conv_general_dilated(u, conv_w[:, None, :], window_strides=(1,),
                                 padding=[(CONV_WIDTH - 1, 0)],
                                 dimension_numbers=('NWC', 'WIO', 'NWC'),
                                 feature_group_count=D_RNN) + conv_b
    ub = u.reshape(B, S, N_RNN_BLOCKS, RNN_BLOCK)
    r = jax.nn.sigmoid(jnp.einsum('bsnc,ncd->bsnd', ub, w_rec).reshape(B, S, D_RNN) + b_rec)
    i = jax.nn.sigmoid(jnp.einsum('bsnc,ncd->bsnd', ub, w_inp).reshape(B, S, D_RNN) + b_inp)
    log_a = (-LRU_C * r.astype(jnp.float32)) * jax.nn.softplus(-lam.astype(jnp.float32))
    a = jnp.exp(log_a)
    b = jnp.sqrt(-jnp.expm1(2.0 * log_a)) * (i * u).astype(jnp.float32)

    def combine(left, right):
        a1, b1 = left
        a2, b2 = right
        return a1 * a2, a2 * b1 + b2

    _, hs = lax.associative_scan(combine, (a, b), axis=1)
    out = hs.astype(h.dtype) * jax.nn.gelu(y)
    return out @ w_out


def shared_kv(x, kv_norm, kv_w, kv_b_forget):
    B, S, _ = x.shape
    HD = N_HEADS * HEAD_DIM
    h = rms_norm(x, kv_norm)
    p = h @ kv_w
    k = p[..., :HD].reshape(B, S, N_HEADS, HEAD_DIM).transpose(0, 2, 1, 3)
    v = p[..., HD:2 * HD].reshape(B, S, N_HEADS, HEAD_DIM).transpose(0, 2, 1, 3)
    f_logit = p[..., 2 * HD:].astype(jnp.float32) + kv_b_forget.astype(jnp.float32)
    cum = jnp.cumsum(jax.nn.log_sigmoid(f_logit), axis=1).transpose(0, 2, 1)
    return k, v, cum


def fox_mixer(h, w_qg, w_o, k, v, cum):
    B, S, _ = h.shape
    HD = N_HEADS * HEAD_DIM
    p = h @ w_qg
    q = p[..., :HD].reshape(B, S, N_HEADS, HEAD_DIM).transpose(0, 2, 1, 3)
    gate = p[..., HD:]
    scale = HEAD_DIM ** -0.5
    k32 = k.astype(jnp.float32)
    v32 = v.astype(jnp.float32)
    key_pos = jnp.arange(S)
    n_blocks = S // Q_BLOCK

    def block(i):
        start = i * Q_BLOCK
        qb = lax.dynamic_slice_in_dim(q, start, Q_BLOCK, axis=2).astype(jnp.float32)
        cb = lax.dynamic_slice_in_dim(cum, start, Q_BLOCK, axis=2)
        s = jnp.einsum('bhqd,bhkd->bhqk', qb, k32) * scale + (cb[..., :, None] - cum[..., None, :])
        q_pos = start + jnp.arange(Q_BLOCK)
        s = jnp.where(key_pos[None, :] <= q_pos[:, None], s, -jnp.inf)
        pr = jax.nn.softmax(s, axis=-1)
        return jnp.einsum('bhqk,bhkd->bhqd', pr, v32)

    o = lax.map(block, jnp.arange(n_blocks))
    o = o.transpose(1, 0, 3, 2, 4).reshape(B, S, HD)
    o = o.astype(h.dtype) * jax.nn.sigmoid(gate)
    return o @ w_o


def hierarchical_moe(h, w_group, b_group, w_router, b_router, w_in, w_out):
    B, S, D = h.shape
    N = B * S
    xf = h.reshape(N, D)
    gl = (xf @ w_group).astype(jnp.float32) + b_group.astype(jnp.float32)
    gp = jax.nn.softmax(gl, axis=-1)
    gp_top, g_idx = lax.top_k(gp, 1)
    el = ((xf @ w_router).astype(jnp.float32) + b_router.astype(jnp.float32)).reshape(N, N_GROUPS, EXPERTS_PER_GROUP)
    el_sel = el[jnp.arange(N), g_idx[:, 0]]
    ep = jax.nn.softmax(el_sel, axis=-1)
    ep_top, e_idx = lax.top_k(ep, TOP_K)
    wts = gp_top * ep_top / jnp.sum(ep_top, axis=-1, keepdims=True)
    eid = (g_idx * EXPERTS_PER_GROUP + e_idx).reshape(-1).astype(jnp.int32)

    A = N * TOP_K
    tok = jnp.repeat(jnp.arange(N, dtype=jnp.int32), TOP_K)
    order = jnp.argsort(eid)
    eid_s = eid[order]
    tok_s = tok[order]
    wt_s = wts.reshape(-1)[order]
    counts = jnp.bincount(eid, length=N_EXPERTS)
    starts = jnp.cumsum(counts) - counts
    padded = (counts + ROW_BLOCK - 1) // ROW_BLOCK * ROW_BLOCK
    pends = jnp.cumsum(padded)
    pstarts = pends - padded
    dest = pstarts[eid_s] + jnp.arange(A, dtype=jnp.int32) - starts[eid_s]
    n_rows = (-(-A // ROW_BLOCK)) * ROW_BLOCK + N_EXPERTS * ROW_BLOCK
    row_tok = jnp.zeros((n_rows,), jnp.int32).at[dest].set(tok_s)
    row_wt = jnp.zeros((n_rows,), wt_s.dtype).at[dest].set(wt_s)
    n_chunks = n_rows // ROW_BLOCK
    chunk_start = jnp.arange(n_chunks, dtype=pends.dtype) * ROW_BLOCK
    chunk_e = jnp.minimum(jnp.searchsorted(pends, chunk_start, side='right'), N_EXPERTS - 1)
    xs = xf[row_tok].reshape(n_chunks, ROW_BLOCK, D)

    def run(args):
        xc, e = args
        gu = xc @ w_in[e]
        return (jax.nn.silu(gu[:, :D_EXPERT]) * gu[:, D_EXPERT:]) @ w_out[e]

    ys = lax.map(run, (xs, chunk_e)).reshape(n_rows, D)
    out = jax.ops.segment_sum(ys * row_wt[:, None].astype(ys.dtype), row_tok, num_segments=N)
    return out.reshape(B, S, D).astype(h.dtype)


def setup_inputs(seed: int = 0) -> dict:
    key = jax.random.key(seed)
    ks = jax.random.split(key, 32)
    f32 = jnp.float32
    HD = N_HEADS * HEAD_DIM

    def nrm(k, shape, scale):
        return jax.random.normal(k, shape, f32) * scale

    x = nrm(ks[0], (BATCH, SEQ, D_MODEL), 1.0)
    a_norm = 1.0 + nrm(ks[1], (N_A_LAYERS, D_MODEL), 0.01)
    a_w_in = nrm(ks[2], (N_A_LAYERS, D_MODEL, 2 * D_RNN), D_MODEL ** -0.5)
    a_conv_w = nrm(ks[3], (N_A_LAYERS, CONV_WIDTH, D_RNN), CONV_WIDTH ** -0.5)
    a_conv_b = nrm(ks[4], (N_A_LAYERS, D_RNN), 0.01)
    a_w_rec = nrm(ks[5], (N_A_LAYERS, N_RNN_BLOCKS, RNN_BLOCK, RNN_BLOCK), RNN_BLOCK ** -0.5)
    a_b_rec = nrm(ks[6], (N_A_LAYERS, D_RNN), 0.01)
    a_w_inp = nrm(ks[7], (N_A_LAYERS, N_RNN_BLOCKS, RNN_BLOCK, RNN_BLOCK), RNN_BLOCK ** -0.5)
    a_b_inp = nrm(ks[8], (N_A_LAYERS, D_RNN), 0.01)
    a_base = jax.random.uniform(ks[9], (N_A_LAYERS, D_RNN), f32, 0.9, 0.999)
    s = a_base ** (1.0 / LRU_C)
    a_lambda = jnp.log(s) - jnp.log1p(-s)
    a_w_out = nrm(ks[10], (N_A_LAYERS, D_RNN, D_MODEL), D_RNN ** -0.5)
    kv_norm = 1.0 + nrm(ks[11], (D_MODEL,), 0.01)
    kv_w = jnp.concatenate([nrm(ks[12], (D_MODEL, 2 * HD), D_MODEL ** -0.5),
                            nrm(ks[13], (D_MODEL, N_HEADS), 0.1 * D_MODEL ** -0.5)], axis=1)
    kv_b_forget = FORGET_BIAS_MEAN + nrm(ks[14], (N_HEADS,), 0.5)
    b_norm = 1.0 + nrm(ks[15], (N_B_LAYERS, D_MODEL), 0.01)
    b_w_qg = nrm(ks[16], (N_B_LAYERS, D_MODEL, 2 * HD), D_MODEL ** -0.5)
    b_w_o = nrm(ks[17], (N_B_LAYERS, HD, D_MODEL), HD ** -0.5)
    m_norm = 1.0 + nrm(ks[18], (DEPTH, D_MODEL), 0.01)
    m_w_group = nrm(ks[19], (DEPTH, D_MODEL, N_GROUPS), D_MODEL ** -0.5)
    m_b_group = nrm(ks[20], (DEPTH, N_GROUPS), 0.01)
    m_w_router = nrm(ks[21], (DEPTH, D_MODEL, N_EXPERTS), D_MODEL ** -0.5)
    m_b_router = nrm(ks[22], (DEPTH, N_EXPERTS), 0.01)
    m_w_in = nrm(ks[23], (DEPTH, N_EXPERTS, D_MODEL, 2 * D_EXPERT), D_MODEL ** -0.5)
    m_w_out = nrm(ks[24], (DEPTH, N_EXPERTS, D_EXPERT, D_MODEL), D_EXPERT ** -0.5)
    final_norm = 1.0 + nrm(ks[25], (D_MODEL,), 0.01)
    return {'x': x, 'a_norm': a_norm, 'a_w_in': a_w_in, 'a_conv_w': a_conv_w, 'a_conv_b': a_conv_b,
            'a_w_rec': a_w_rec, 'a_b_rec': a_b_rec, 'a_w_inp': a_w_inp, 'a_b_inp': a_b_inp,
            'a_lambda': a_lambda, 'a_w_out': a_w_out, 'kv_norm': kv_norm, 'kv_w': kv_w,
            'kv_b_forget': kv_b_forget, 'b_norm': b_norm, 'b_w_qg': b_w_qg, 'b_w_o': b_w_o,
            'm_norm': m_norm, 'm_w_group': m_w_group, 'm_b_group': m_b_group,
            'm_w_router': m_w_router, 'm_b_router': m_b_router, 'm_w_in': m_w_in,
            'm_w_out': m_w_out, 'final_norm': final_norm}


def reference(x, a_norm, a_w_in, a_conv_w, a_conv_b, a_w_rec, a_b_rec, a_w_inp, a_b_inp,
              a_lambda, a_w_out, kv_norm, kv_w, kv_b_forget, b_norm, b_w_qg, b_w_o,
              m_norm, m_w_group, m_b_group, m_w_router, m_b_router, m_w_in, m_w_out,
              final_norm):
    k = v = cum = None
    for layer in range(DEPTH):
        if layer < N_A_LAYERS:
            i = layer
            x = x + rglru_mixer(rms_norm(x, a_norm[i]), a_w_in[i], a_conv_w[i], a_conv_b[i],
                                a_w_rec[i], a_b_rec[i], a_w_inp[i], a_b_inp[i],
                                a_lambda[i], a_w_out[i])
        else:
            j = layer - N_A_LAYERS
            x = x + fox_mixer(rms_norm(x, b_norm[j]), b_w_qg[j], b_w_o[j], k, v, cum)
        x = x + hierarchical_moe(rms_norm(x, m_norm[layer]), m_w_group[layer], m_b_group[layer],
                                 m_w_router[layer], m_b_router[layer], m_w_in[layer], m_w_out[layer])
        if layer == N_A_LAYERS - 1:
            k, v, cum = shared_kv(x, kv_norm, kv_w, kv_b_forget)
    return rms_norm(x, final_norm)
```

```python
from contextlib import ExitStack
import numpy as np
import concourse.bass as bass
import concourse.mybir as mybir

F32 = mybir.dt.float32
BF16 = mybir.dt.bfloat16
I32 = mybir.dt.int32
U32 = mybir.dt.uint32
ALU = mybir.AluOpType
AF = mybir.ActivationFunctionType
AX = mybir.AxisListType

ENGS = ("pe", "act", "dve", "pool", "sp")


class Prog:
    def __init__(self, nc, es: ExitStack):
        self.nc = nc
        self.es = es
        self.streams = {e: [] for e in ENGS}
        self.es_root = es
        self.esem = {}
        self.epoch = {e: 0 for e in ENGS}
        self.efinal = {}
        for e in ENGS:
            self.esem[(e, 0)] = es.enter_context(nc.semaphore("s_" + e))
        self.cnt = {e: 0 for e in ENGS}
        self.dsem = {}
        self.dcnt = {}
        self.waited = {e: {} for e in ENGS}
        self.state = {}
        self.nsem = len(ENGS)

    def arena_init(self, sb_bytes=188 * 1024):
        self._sb = self.es.enter_context(self.nc.sbuf_tensor("arena", [128, sb_bytes // 4], F32))
        self._ps = self.es.enter_context(self.nc.psum_tensor("parena", [128, 4096], F32))
        self._sb_off = 0
        self._sb_cap = sb_bytes // 4
        self._ps_off = 0

    def sb(self, name, shape, dt):
        if not hasattr(self, "_sb"):
            self.arena_init()
        esz = mybir.dt.size(dt)
        n = int(np.prod(shape[1:]))
        words = (n * esz + 31) // 32 * 8
        assert self._sb_off + words <= self._sb_cap, ("SBUF arena overflow", name, self._sb_off * 4, words * 4)
        v = self._sb[0:shape[0], self._sb_off:self._sb_off + (n * esz + 3) // 4]
        self._sb_off += words
        if dt != F32:
            v = v.bitcast(dt)
        v = v[:, 0:n]
        if len(shape) == 3:
            v = v.rearrange("p (a b) -> p a b", a=shape[1])
        return v

    def ps(self, name, shape, dt):
        if not hasattr(self, "_sb"):
            self.arena_init()
        esz = mybir.dt.size(dt)
        n = int(np.prod(shape[1:]))
        words = (n * esz + 2047) // 2048 * 512
        assert self._ps_off + words <= 4096, ("PSUM arena overflow", name)
        v = self._ps[0:shape[0], self._ps_off:self._ps_off + (n * esz + 3) // 4]
        self._ps_off += words
        if dt != F32:
            v = v.bitcast(dt)
        v = v[:, 0:n]
        if len(shape) == 3:
            v = v.rearrange("p (a b) -> p a b", a=shape[1])
        return v

    def scope(self):
        from contextlib import contextmanager

        @contextmanager
        def _cm():
            if not hasattr(self, "_sb"):
                self.arena_init()
            so, po = self._sb_off, self._ps_off
            self.barrier()
            try:
                yield
            finally:
                self.barrier()
                self._sb_off, self._ps_off = so, po
        return _cm()

    def _dma_sem(self, key):
        if key not in self.dsem:
            self.dsem[key] = self.es_root.enter_context(self.nc.semaphore("d_%d" % len(self.dsem)))
            self.dcnt[key] = 0
            self.nsem += 1
        return self.dsem[key]

    def _deps(self, reads, writes):
        evs = []
        for b in list(reads) + list(writes):
            st = self.state.get(b)
            if st and st[0] is not None:
                evs.append(st[0])
        for b in writes:
            st = self.state.get(b)
            if st:
                evs.extend(st[1])
        return evs

    def _record(self, ev, reads, writes):
        for b in reads:
            st = self.state.setdefault(b, [None, []])
            st[1].append(ev)
        for b in writes:
            self.state[b] = [ev, []]

    def _emit_waits(self, eng, evs):
        need = {}
        for (sk, v) in evs:
            if sk[0] == "d":
                v = max(v, self.dcnt[sk[1]])
            if v > need.get(sk, 0):
                need[sk] = v
        for sk, v in need.items():
            if self.waited[eng].get(sk, 0) >= v:
                continue
            self.waited[eng][sk] = v
            sem = self.esem[(sk[1], sk[2])] if sk[0] == "e" else self.dsem[sk[1]]
            self.streams[eng].append(lambda e, sem=sem, v=v: e.wait_ge(sem, v))

    def op(self, eng, fn, reads=(), writes=()):
        evs = self._deps(reads, writes)
        self._emit_waits(eng, evs)
        if self.cnt[eng] >= 50000:
            self.efinal[(eng, self.epoch[eng])] = self.cnt[eng]
            self.epoch[eng] += 1
            self.cnt[eng] = 0
            self.esem[(eng, self.epoch[eng])] = self.es_root.enter_context(self.nc.semaphore("s_%s%d" % (eng, self.epoch[eng])))
            self.nsem += 1
        self.cnt[eng] += 1
        ev = (("e", eng, self.epoch[eng]), self.cnt[eng])
        sem = self.esem[(eng, self.epoch[eng])]
        self.streams[eng].append(lambda e, fn=fn, sem=sem: fn(e).then_inc(sem, 1))
        self._record(ev, reads, writes)
        return ev

    def dma(self, q, fn, key, reads=(), writes=()):
        sem = self._dma_sem(key)
        evs = self._deps(reads, writes)
        self._emit_waits(q, evs)
        self.dcnt[key] += 16
        ev = (("d", key), self.dcnt[key])
        self.streams[q].append(lambda e, fn=fn, sem=sem: fn(e).then_inc(sem, 16))
        self._record(ev, reads, writes)
        return ev

    def wait_all(self, eng="sp"):
        evs = []
        for k, c in self.dcnt.items():
            if c:
                evs.append((("d", k), c))
        for e in ENGS:
            if self.cnt[e]:
                evs.append((("e", e, self.epoch[e]), self.cnt[e]))
        for (e, ep), c in self.efinal.items():
            evs.append((("e", e, ep), c))
        self._emit_waits(eng, evs)

    def barrier(self):
        for e in ENGS:
            self.wait_all(e)

    def run(self):
        nc = self.nc
        with nc.Block() as block:
            @block.tensor
            def _(e):
                for f in self.streams["pe"]:
                    f(e)

            @block.scalar
            def _(e):
                for f in self.streams["act"]:
                    f(e)

            @block.vector
            def _(e):
                for f in self.streams["dve"]:
                    f(e)

            @block.gpsimd
            def _(e):
                for f in self.streams["pool"]:
                    f(e)

            @block.sync
            def _(e):
                for f in self.streams["sp"]:
                    f(e)


from concourse.bass_utils import run_bass_kernel_spmd

EPS = 1e-6
LRU_C = 8.0
GK = 1.5957691216057308
NG, EPG, NE = 8, 8, 64
NL = NG + NE
DFF = 512
CAP = 768
RG = 256
BIG = 30000.0
HD = 128

def emit_rglru(P, uT, yT, outT, conv_w, conv_b, w_rec, b_rec, w_inp, b_inp, lam, NSEQ, S, pfx="r"):
    nc = P.nc
    CH = 1024
    nch = S // CH
    cw = P.sb(f"{pfx}_cw", [128, 2, 4], F32)
    cb = P.sb(f"{pfx}_cb", [128, 2], F32)
    br = P.sb(f"{pfx}_br", [128, 2], F32)
    bi = P.sb(f"{pfx}_bi", [128, 2], F32)
    lm = P.sb(f"{pfx}_lm", [128, 2], F32)
    c1 = P.sb(f"{pfx}_c1", [128, 2], F32)
    c2 = P.sb(f"{pfx}_c2", [128, 2], F32)
    wrf = P.sb(f"{pfx}_wrf", [128, 2, 256], F32)
    wif = P.sb(f"{pfx}_wif", [128, 2, 256], F32)
    wrb = P.sb(f"{pfx}_wrb", [128, 2, 256], BF16)
    wib = P.sb(f"{pfx}_wib", [128, 2, 256], BF16)
    KC = f"{pfx}_const"
    for ft in range(2):
        for j in range(4):
            P.dma("sp", lambda e, ft=ft, j=j: e.dma_start(
                out=cw[:, ft, j:j + 1], in_=conv_w[j, ft * 128:(ft + 1) * 128].rearrange("(p o) -> p o", o=1)),
                key=KC, writes=[KC])
        for (dst, src) in ((cb, conv_b), (br, b_rec), (bi, b_inp), (lm, lam)):
            P.dma("sp", lambda e, ft=ft, dst=dst, src=src: e.dma_start(
                out=dst[:, ft:ft + 1], in_=src[ft * 128:(ft + 1) * 128].rearrange("(p o) -> p o", o=1)),
                key=KC, writes=[KC])
    P.dma("sp", lambda e: e.dma_start(out=wrf[:], in_=w_rec.rearrange("(k p) n -> p k n", p=128)), key=KC, writes=[KC])
    P.dma("sp", lambda e: e.dma_start(out=wif[:], in_=w_inp.rearrange("(k p) n -> p k n", p=128)), key=KC, writes=[KC])
    P.op("dve", lambda e: e.tensor_copy(out=wrb[:], in_=wrf[:]), reads=[KC], writes=[KC + "b"])
    P.op("dve", lambda e: e.tensor_copy(out=wib[:], in_=wif[:]), reads=[KC], writes=[KC + "b2"])
    t0 = P.sb(f"{pfx}_t0", [128, 2], F32)
    t1 = P.sb(f"{pfx}_t1", [128, 2], F32)
    t2 = P.sb(f"{pfx}_t2", [128, 2], F32)
    t3 = P.sb(f"{pfx}_t3", [128, 2], F32)
    t4 = P.sb(f"{pfx}_t4", [128, 2], F32)
    KS = f"{pfx}_sp"
    P.op("dve", lambda e: e.tensor_scalar_mul(out=t0[:], in0=lm[:], scalar1=-1.0), reads=[KC], writes=[KS + "0"])
    P.op("dve", lambda e: e.tensor_tensor(out=t0[:], in0=t0[:], in1=lm[:], op=ALU.max), reads=[KC, KS + "0"], writes=[KS + "0"])
    P.op("act", lambda e: e.activation(out=t1[:], in_=t0[:], func=AF.Exp, scale=-1.0), reads=[KS + "0"], writes=[KS + "1"])
    P.op("dve", lambda e: e.tensor_scalar_add(out=t2[:], in0=t1[:], scalar1=2.0), reads=[KS + "1"], writes=[KS + "2"])
    P.op("dve", lambda e: e.reciprocal(out=t2[:], in_=t2[:]), reads=[KS + "2"], writes=[KS + "2"])
    P.op("dve", lambda e: e.tensor_tensor(out=t2[:], in0=t2[:], in1=t1[:], op=ALU.mult), reads=[KS + "2", KS + "1"], writes=[KS + "2"])
    P.op("dve", lambda e: e.tensor_tensor(out=t3[:], in0=t2[:], in1=t2[:], op=ALU.mult), reads=[KS + "2"], writes=[KS + "3"])
    P.op("dve", lambda e: e.memset(t4[:], 1.0 / 19.0), writes=[KS + "4"])
    for n in (17, 15, 13, 11, 9, 7, 5, 3, 1):
        P.op("dve", lambda e: e.tensor_tensor(out=t4[:], in0=t4[:], in1=t3[:], op=ALU.mult), reads=[KS + "4", KS + "3"], writes=[KS + "4"])
        P.op("dve", lambda e, n=n: e.tensor_scalar_add(out=t4[:], in0=t4[:], scalar1=1.0 / n), reads=[KS + "4"], writes=[KS + "4"])
    P.op("dve", lambda e: e.tensor_tensor(out=t4[:], in0=t4[:], in1=t2[:], op=ALU.mult), reads=[KS + "4", KS + "2"], writes=[KS + "4"])
    P.op("dve", lambda e: e.tensor_scalar(out=t0[:], in0=lm[:], scalar1=-1.0, scalar2=0.0, op0=ALU.mult, op1=ALU.max),
         reads=[KC, KS + "0"], writes=[KS + "0"])
    P.op("dve", lambda e: e.scalar_tensor_tensor(out=t0[:], in0=t4[:], scalar=2.0, in1=t0[:], op0=ALU.mult, op1=ALU.add),
         reads=[KS + "4", KS + "0"], writes=[KS + "0"])
    P.op("dve", lambda e: e.tensor_scalar_mul(out=c1[:], in0=t0[:], scalar1=-LRU_C), reads=[KS + "0"], writes=[KS + "c1"])
    P.op("dve", lambda e: e.tensor_scalar_mul(out=c2[:], in0=t0[:], scalar1=-2.0 * LRU_C), reads=[KS + "0"], writes=[KS + "c2"])
    CONST = [KC, KC + "b", KC + "b2", KS + "c1", KS + "c2"]

    NB = 2
    def mk(name, dt=F32, w=CH):
        return [[P.sb(f"{pfx}_{name}{ft}{b}", [128, w], dt) for b in range(NB)] for ft in range(2)]
    ut = mk("ut", F32, CH + 3)
    uc = mk("uc")
    ucb = mk("ucb", BF16)
    yt = mk("yt")
    rr = mk("rr")
    ii = mk("ii")
    aa = mk("aa")
    sq = mk("sq")
    hs = mk("hs")
    g1 = mk("g1")
    ot = mk("ot")
    prs = [[P.ps(f"{pfx}_pr{j}{b}", [128, CH], F32) for b in range(1)] for j in range(2)]
    pis = [[P.ps(f"{pfx}_pi{j}{b}", [128, CH], F32) for b in range(1)] for j in range(2)]

    it = 0
    for s in range(NSEQ):
        for c in range(nch):
            b = it % NB
            pb = (it - 1) % NB
            it += 1
            col0 = s * S + c * CH
            for ft in range(2):
                kut = f"{pfx}_ut{ft}{b}"
                if c == 0:
                    P.op("pool", lambda e, ft=ft, b=b: e.memset(ut[ft][b][:, 0:3], 0.0), writes=[kut])
                    P.dma("sp", lambda e, ft=ft, b=b, col0=col0: e.dma_start(
                        out=ut[ft][b][:, 3:], in_=uT[ft * 128:(ft + 1) * 128, col0:col0 + CH]), key=kut, writes=[kut], reads=[kut])
                else:
                    P.dma("sp", lambda e, ft=ft, b=b, col0=col0: e.dma_start(
                        out=ut[ft][b][:], in_=uT[ft * 128:(ft + 1) * 128, col0 - 3:col0 + CH]), key=kut, writes=[kut])
                kuc = f"{pfx}_uc{ft}{b}"
                P.op("dve", lambda e, ft=ft, b=b: e.tensor_scalar(
                    out=uc[ft][b][:], in0=ut[ft][b][:, 0:CH], scalar1=cw[:, ft, 0:1], scalar2=cb[:, ft:ft + 1],
                    op0=ALU.mult, op1=ALU.add), reads=[kut] + CONST, writes=[kuc])
                for j in range(1, 4):
                    P.op("dve", lambda e, ft=ft, b=b, j=j: e.scalar_tensor_tensor(
                        out=uc[ft][b][:], in0=ut[ft][b][:, j:j + CH], scalar=cw[:, ft, j:j + 1], in1=uc[ft][b][:],
                        op0=ALU.mult, op1=ALU.add), reads=[kut, kuc] + CONST, writes=[kuc])
                P.op("pool", lambda e, ft=ft, b=b: e.tensor_copy(out=ucb[ft][b][:], in_=uc[ft][b][:]),
                     reads=[kuc], writes=[f"{pfx}_ucb{ft}{b}"])
                kyt = f"{pfx}_yt{ft}{b}"
                P.dma("sp", lambda e, ft=ft, b=b, col0=col0: e.dma_start(
                    out=yt[ft][b][:], in_=yT[ft * 128:(ft + 1) * 128, col0:col0 + CH]), key=kyt, writes=[kyt])
            for j in range(2):
                kpr = f"{pfx}_pr{j}"
                kpi = f"{pfx}_pi{j}"

                def mmg(e, j=j, b=b, w=wrb, pp=prs):
                    last = None
                    for q in range(CH // 512):
                        for kt in range(2):
                            last = e.matmul(pp[j][0][:, q * 512:(q + 1) * 512], lhsT=w[:, kt, j * 128:(j + 1) * 128],
                                            rhs=ucb[kt][b][:, q * 512:(q + 1) * 512], start=(kt == 0), stop=(kt == 1))
                    return last
                P.op("pe", mmg, reads=[f"{pfx}_ucb0{b}", f"{pfx}_ucb1{b}"] + CONST, writes=[kpr])
                P.op("pe", lambda e, j=j, b=b: mmg(e, j, b, wib, pis),
                     reads=[f"{pfx}_ucb0{b}", f"{pfx}_ucb1{b}"] + CONST, writes=[kpi])
                K = lambda n: f"{pfx}_{n}{j}{b}"
                P.op("act", lambda e, j=j, b=b: e.activation(out=rr[j][b][:], in_=prs[j][0][:], func=AF.Sigmoid,
                                                            bias=br[:, j:j + 1]), reads=[kpr] + CONST, writes=[K("rr")])
                P.op("act", lambda e, j=j, b=b: e.activation(out=ii[j][b][:], in_=pis[j][0][:], func=AF.Sigmoid,
                                                            bias=bi[:, j:j + 1]), reads=[kpi] + CONST, writes=[K("ii")])
                P.op("act", lambda e, j=j, b=b: e.activation(out=aa[j][b][:], in_=rr[j][b][:], func=AF.Exp,
                                                            scale=c1[:, j:j + 1]), reads=[K("rr")] + CONST, writes=[K("aa")])
                P.op("act", lambda e, j=j, b=b: e.activation(out=sq[j][b][:], in_=rr[j][b][:], func=AF.Exp,
                                                            scale=c2[:, j:j + 1]), reads=[K("rr")] + CONST, writes=[K("sq")])
                P.op("act", lambda e, j=j, b=b: e.activation(out=sq[j][b][:], in_=sq[j][b][:], func=AF.Sqrt,
                                                            scale=-1.0, bias=ONE_AP[0][:]), reads=[K("sq"), "c_ones"], writes=[K("sq")])
                P.op("dve", lambda e, j=j, b=b: e.tensor_tensor(out=ii[j][b][:], in0=ii[j][b][:], in1=uc[j][b][:], op=ALU.mult),
                     reads=[K("ii"), f"{pfx}_uc{j}{b}"], writes=[K("ii")])
                P.op("dve", lambda e, j=j, b=b: e.tensor_tensor(out=ii[j][b][:], in0=ii[j][b][:], in1=sq[j][b][:], op=ALU.mult),
                     reads=[K("ii"), K("sq")], writes=[K("ii")])
                if c == 0:
                    P.op("dve", lambda e, j=j, b=b: e.tensor_tensor_scan(
                        out=hs[j][b][:], data0=aa[j][b][:], data1=ii[j][b][:], initial=0.0, op0=ALU.mult, op1=ALU.add),
                        reads=[K("aa"), K("ii")], writes=[K("hs")])
                else:
                    P.op("dve", lambda e, j=j, b=b, pb=pb: e.tensor_tensor_scan(
                        out=hs[j][b][:], data0=aa[j][b][:], data1=ii[j][b][:], initial=hs[j][pb][:, CH - 1:CH],
                        op0=ALU.mult, op1=ALU.add),
                        reads=[K("aa"), K("ii"), f"{pfx}_hs{j}{pb}"], writes=[K("hs")])
                kyt = f"{pfx}_yt{j}{b}"
                P.op("pool", lambda e, j=j, b=b: e.tensor_tensor(out=g1[j][b][:], in0=yt[j][b][:], in1=yt[j][b][:], op=ALU.mult),
                     reads=[kyt], writes=[K("g1")])
                P.op("pool", lambda e, j=j, b=b: e.tensor_scalar(out=g1[j][b][:], in0=g1[j][b][:], scalar1=0.044715, scalar2=1.0,
                                                                op0=ALU.mult, op1=ALU.add), reads=[K("g1")], writes=[K("g1")])
                P.op("pool", lambda e, j=j, b=b: e.tensor_tensor(out=g1[j][b][:], in0=g1[j][b][:], in1=yt[j][b][:], op=ALU.mult),
                     reads=[K("g1"), kyt], writes=[K("g1")])
                P.op("act", lambda e, j=j, b=b: e.activation(out=g1[j][b][:], in_=g1[j][b][:], func=AF.Sigmoid, scale=GK),
                     reads=[K("g1")], writes=[K("g1")])
                P.op("pool", lambda e, j=j, b=b: e.tensor_tensor(out=g1[j][b][:], in0=g1[j][b][:], in1=yt[j][b][:], op=ALU.mult),
                     reads=[K("g1"), kyt], writes=[K("g1")])
                P.op("dve", lambda e, j=j, b=b: e.tensor_tensor(out=ot[j][b][:], in0=g1[j][b][:], in1=hs[j][b][:], op=ALU.mult),
                     reads=[K("g1"), K("hs")], writes=[K("ot")])
                P.dma("sp", lambda e, j=j, b=b, col0=col0: e.dma_start(
                    out=outT[j * 128:(j + 1) * 128, col0:col0 + CH], in_=ot[j][b][:]), key=K("ot") + "_st", reads=[K("ot")])


ONE_AP = [None]


def emit_one(P):
    one = P.sb("one", [128, 1], F32)
    P.op("pool", lambda e: e.memset(one[:], 1.0), writes=["one"])
    ONE_AP[0] = one


_BC = {}


def _bc_reg(e, val):
    k = (id(e), val)
    if k not in _BC:
        _BC[k] = e.to_reg(val)
    return _BC[k]


def emit_moe_consts(P):
    c = {}
    idf = P.sb("c_idf", [128, 128], F32)
    idb = P.sb("c_idb", [128, 128], BF16)
    U = P.sb("c_U", [128, 128], F32)
    ones = P.sb("c_ones", [128, 128], F32)
    eps = P.sb("c_eps", [128, 1], F32)
    base = P.sb("c_base", [128, NE], F32)
    basei = P.sb("c_basei", [128, NE], I32)
    P.op("pool", lambda e: e.memset(idf[:], 0.0), writes=["c_idf"])
    P.op("pool", lambda e: e.affine_select(out=idf[:], in_=idf[:], pattern=[[-1, 128]], compare_op=ALU.not_equal,
                                           fill=1.0, base=0, channel_multiplier=1), reads=["c_idf"], writes=["c_idf"])
    P.op("pool", lambda e: e.tensor_copy(out=idb[:], in_=idf[:]), reads=["c_idf"], writes=["c_idb"])
    P.op("pool", lambda e: e.memset(ones[:], 1.0), writes=["c_ones"])
    P.op("pool", lambda e: e.memset(eps[:], EPS), writes=["c_eps"])
    P.op("pool", lambda e: e.affine_select(out=U[:], in_=ones[:], pattern=[[1, 128]], compare_op=ALU.is_gt,
                                           fill=0.0, base=0, channel_multiplier=-1), reads=["c_ones"], writes=["c_U"])
    P.op("pool", lambda e: e.iota(basei[:], pattern=[[CAP, NE]], base=0, channel_multiplier=0), writes=["c_basei"])
    P.op("pool", lambda e: e.tensor_copy(out=base[:], in_=basei[:]), reads=["c_basei"], writes=["c_base"])
    E0 = P.sb("c_E0", [128, 128], F32)
    P.op("pool", lambda e: e.memset(E0[:], 0.0), writes=["c_E0"])
    P.op("pool", lambda e: e.memset(E0[0:1, :], 1.0), reads=["c_E0"], writes=["c_E0"])
    c.update(idf=idf, idb=idb, U=U, ones=ones, eps=eps, base=base, E0=E0)
    c["keys"] = ["c_idf", "c_idb", "c_U", "c_ones", "c_eps", "c_base", "c_E0"]
    return c


def emit_moe(P, C, x, xo, gain, w_group, b_group, w_router, b_router, w_in, w_out, xs, ys, T, D=2048, pfx="m"):
    nc = P.nc
    KT = D // 128
    NTILE = T // 128
    NROWS = NE * CAP
    CK = C["keys"]
    dst = P.sb(f"{pfx}_dst", [128, NTILE, 2], I32)
    wts = P.sb(f"{pfx}_wts", [128, NTILE, 2], F32)
    carry = P.sb(f"{pfx}_carry", [128, NE], F32)
    P.op("dve", lambda e: e.memset(carry[:], 0.0), writes=[f"{pfx}_carry"])

    with P.scope():
        gbc = P.sb(f"{pfx}_gbc", [128, D], F32)
        bt = P.sb(f"{pfx}_bt", [128, NL], F32)
        Wr = P.sb(f"{pfx}_Wr", [128, KT, NL], F32)
        K1 = f"{pfx}_p1c"
        P.dma("sp", lambda e: e.dma_start(out=gbc[:], in_=gain.partition_broadcast(128)), key=K1, writes=[K1])
        P.dma("sp", lambda e: e.dma_start(out=bt[:, 0:NG], in_=b_group.partition_broadcast(128)), key=K1, writes=[K1])
        P.dma("sp", lambda e: e.dma_start(out=bt[:, NG:NL], in_=b_router.partition_broadcast(128)), key=K1, writes=[K1])
        P.dma("sp", lambda e: e.dma_start(out=Wr[:, :, 0:NG], in_=w_group.rearrange("(k p) n -> p k n", p=128)), key=K1, writes=[K1])
        P.dma("sp", lambda e: e.dma_start(out=Wr[:, :, NG:NL], in_=w_router.rearrange("(k p) n -> p k n", p=128)), key=K1, writes=[K1])
        xt = [P.sb(f"{pfx}_xt{i}", [128, D], F32) for i in range(2)]
        hf = [P.sb(f"{pfx}_hf{i}", [128, D], F32) for i in range(2)]
        hb = [P.sb(f"{pfx}_hb{i}", [128, D], BF16) for i in range(2)]
        junk = P.sb(f"{pfx}_junk", [128, D], BF16)
        hTf = P.sb(f"{pfx}_hTf", [128, KT, 128], F32)
        pTf = P.ps(f"{pfx}_pTf", [128, D], F32)
        lgp = P.ps(f"{pfx}_lgp", [128, NL], F32)
        rkp = P.ps(f"{pfx}_rkp", [128, NE], F32)
        csp = P.ps(f"{pfx}_csp", [128, NE], F32)
        sm = {}
        for n, w in (("ss", 1), ("sd", 1), ("rs", 1), ("lg", NL), ("gmax", 1), ("ngmax", 1), ("ohg", NG), ("eg", NG),
                     ("gsum", 1), ("gp", 1), ("pen", NG), ("me", NE), ("top", 8), ("oh1", NE), ("oh2", NE), ("d", 1),
                     ("w1", 1), ("M", NE), ("pos", NE), ("ovf", NE), ("tmp", NE), ("df", 2)):
            sm[n] = P.sb(f"{pfx}_s_{n}", [128, w], F32)
        for i in range(NTILE):
            s = i % 2
            r0 = i * 128
            kx, khf, khb = f"{pfx}_xt{s}", f"{pfx}_hf{s}", f"{pfx}_hb{s}"
            S = lambda n: f"{pfx}_s_{n}"
            P.dma("sp", lambda e, s=s, r0=r0: e.dma_start(out=xt[s][:], in_=x[r0:r0 + 128, :]), key=kx, writes=[kx])
            P.op("act", lambda e, s=s: e.activation(out=junk[:], in_=xt[s][:], func=AF.Square, accum_out=sm["ss"][:]),
                 reads=[kx], writes=[f"{pfx}_junk", S("ss")])
            P.op("act", lambda e: e.activation(out=sm["sd"][:], in_=sm["ss"][:], func=AF.Sqrt, scale=1.0 / D, bias=C["eps"][:]),
                 reads=[S("ss")] + CK, writes=[S("sd")])
            P.op("dve", lambda e: e.reciprocal(out=sm["rs"][:], in_=sm["sd"][:]), reads=[S("sd")], writes=[S("rs")])
            P.op("dve", lambda e, s=s: e.scalar_tensor_tensor(out=hf[s][:], in0=xt[s][:], scalar=sm["rs"][:], in1=gbc[:],
                                                             op0=ALU.mult, op1=ALU.mult), reads=[kx, S("rs"), K1], writes=[khf])
            P.op("act", lambda e, s=s: e.activation(out=hb[s][:], in_=hf[s][:], func=AF.Copy), reads=[khf], writes=[khb])

            def tr(e, s=s):
                last = None
                for k in range(KT):
                    last = e.transpose(out=pTf[:, k * 128:(k + 1) * 128], in_=hf[s][:, k * 128:(k + 1) * 128], identity=C["idf"][:])
                return last
            P.op("pe", tr, reads=[khf] + CK, writes=[f"{pfx}_pTf"])
            P.op("act", lambda e: e.activation(out=hTf[:].rearrange("p k t -> p (k t)"), in_=pTf[:], func=AF.Copy),
                 reads=[f"{pfx}_pTf"], writes=[f"{pfx}_hTf"])

            def mml(e):
                last = None
                for k in range(KT):
                    last = e.matmul(lgp[:], lhsT=hTf[:, k, :], rhs=Wr[:, k, :], start=(k == 0), stop=(k == KT - 1))
                return last
            P.op("pe", mml, reads=[f"{pfx}_hTf", K1], writes=[f"{pfx}_lgp"])
            V = lambda fn, reads, writes: P.op("dve", fn, reads=[(r[1] if isinstance(r, tuple) else S(r)) for r in reads],
                                               writes=[S(w) for w in writes])
            V(lambda e: e.tensor_tensor(out=sm["lg"][:], in0=lgp[:], in1=bt[:], op=ALU.add), [("K", f"{pfx}_lgp"), ("K", K1)], ["lg"])
            V(lambda e: e.reduce_max(out=sm["gmax"][:], in_=sm["lg"][:, 0:NG], axis=AX.X), ["lg"], ["gmax"])
            V(lambda e: e.tensor_scalar(out=sm["ohg"][:], in0=sm["lg"][:, 0:NG], scalar1=sm["gmax"][:], scalar2=None, op0=ALU.is_equal),
              ["lg", "gmax"], ["ohg"])
            V(lambda e: e.tensor_scalar_mul(out=sm["ngmax"][:], in0=sm["gmax"][:], scalar1=-1.0), ["gmax"], ["ngmax"])
            P.op("act", lambda e: e.activation(out=sm["eg"][:], in_=sm["lg"][:, 0:NG], func=AF.Exp, bias=sm["ngmax"][:],
                                               accum_out=sm["gsum"][:]), reads=[S("lg"), S("ngmax")], writes=[S("eg"), S("gsum")])
            V(lambda e: e.reciprocal(out=sm["gp"][:], in_=sm["gsum"][:]), ["gsum"], ["gp"])
            V(lambda e: e.tensor_scalar(out=sm["pen"][:], in0=sm["ohg"][:], scalar1=BIG, scalar2=-BIG, op0=ALU.mult, op1=ALU.add),
              ["ohg"], ["pen"])
            V(lambda e: e.tensor_tensor(out=sm["me"][:].rearrange("p (g e) -> p g e", g=NG),
                                        in0=sm["lg"][:, NG:NL].rearrange("p (g e) -> p g e", g=NG),
                                        in1=sm["pen"][:].unsqueeze(2).to_broadcast([128, NG, EPG]), op=ALU.add), ["lg", "pen"], ["me"])
            V(lambda e: e.max(out=sm["top"][:], in_=sm["me"][:]), ["me"], ["top"])
            V(lambda e: e.tensor_scalar(out=sm["oh1"][:], in0=sm["me"][:], scalar1=sm["top"][:, 0:1], scalar2=None, op0=ALU.is_equal),
              ["me", "top"], ["oh1"])
            V(lambda e: e.tensor_scalar(out=sm["oh2"][:], in0=sm["me"][:], scalar1=sm["top"][:, 1:2], scalar2=None, op0=ALU.is_equal),
              ["me", "top"], ["oh2"])
            V(lambda e: e.tensor_tensor(out=sm["d"][:], in0=sm["top"][:, 0:1], in1=sm["top"][:, 1:2], op=ALU.subtract), ["top"], ["d"])
            P.op("act", lambda e: e.activation(out=sm["w1"][:], in_=sm["d"][:], func=AF.Sigmoid), reads=[S("d")], writes=[S("w1")])
            V(lambda e, i=i: e.tensor_tensor(out=wts[:, i, 0:1], in0=sm["w1"][:], in1=sm["gp"][:], op=ALU.mult), ["w1", "gp"], ["wts0"])
            V(lambda e, i=i: e.tensor_tensor(out=wts[:, i, 1:2], in0=sm["gp"][:], in1=wts[:, i, 0:1], op=ALU.subtract), ["gp", "wts0"], ["wts1"])
            V(lambda e: e.tensor_tensor(out=sm["M"][:], in0=sm["oh1"][:], in1=sm["oh2"][:], op=ALU.add), ["oh1", "oh2"], ["M"])

            def mmr(e):
                e.matmul(rkp[:], lhsT=C["U"][:], rhs=sm["M"][:], start=True, stop=False)
                return e.matmul(rkp[:], lhsT=C["E0"][:], rhs=carry[:], start=False, stop=True)
            P.op("pe", mmr, reads=[S("M"), f"{pfx}_carry"] + CK, writes=[f"{pfx}_rkp"])
            P.op("pe", lambda e: e.matmul(csp[:], lhsT=C["ones"][:], rhs=sm["M"][:], start=True, stop=True),
                 reads=[S("M")] + CK, writes=[f"{pfx}_csp"])
            V(lambda e: e.tensor_scalar(out=sm["ovf"][:], in0=rkp[:], scalar1=float(CAP), scalar2=1.0e7, op0=ALU.is_ge, op1=ALU.mult),
              [("K", f"{pfx}_rkp")], ["ovf"])
            V(lambda e: e.tensor_tensor(out=sm["pos"][:], in0=rkp[:], in1=C["base"][:], op=ALU.add), [("K", f"{pfx}_rkp"), ("K", "c_base")], ["pos"])
            V(lambda e: e.tensor_tensor(out=sm["pos"][:], in0=sm["pos"][:], in1=sm["ovf"][:], op=ALU.add), ["pos", "ovf"], ["pos"])
            P.op("dve", lambda e: e.tensor_tensor(out=carry[:], in0=carry[:], in1=csp[:], op=ALU.add),
                 reads=[f"{pfx}_carry", f"{pfx}_csp"], writes=[f"{pfx}_carry"])
            for k2, oh in ((0, "oh1"), (1, "oh2")):
                V(lambda e, oh=oh: e.tensor_tensor(out=sm["tmp"][:], in0=sm[oh][:], in1=sm["pos"][:], op=ALU.mult), [oh, "pos"], ["tmp"])
                V(lambda e, k2=k2: e.reduce_sum(out=sm["df"][:, k2:k2 + 1], in_=sm["tmp"][:], axis=AX.X), ["tmp"], ["df%d" % k2])
            P.op("dve", lambda e, i=i: e.tensor_copy(out=dst[:, i, :], in_=sm["df"][:]), reads=[S("df0"), S("df1")], writes=[(f"{pfx}_dst", i)])
            for k2 in range(2):
                P.dma("pool", lambda e, s=s, i=i, k2=k2: e.indirect_dma_start(
                    out=xs[:, :], out_offset=bass.IndirectOffsetOnAxis(ap=dst[:, i, k2:k2 + 1], axis=0),
                    in_=hb[s][:, :], in_offset=None, bounds_check=_bc_reg(e, NROWS - 1), oob_is_err=False),
                    key=f"{pfx}_sc{s}{k2}", reads=[khb, (f"{pfx}_dst", i)])
    with P.scope():
        w1 = [P.sb(f"{pfx}_w1{i}", [128, KT, 2 * DFF], BF16) for i in range(2)]
        w2 = [P.sb(f"{pfx}_w2{i}", [128, DFF // 128, D], BF16) for i in range(2)]
        xr = [P.sb(f"{pfx}_xr{i}", [128, D], BF16) for i in range(4)]
        xsT = [P.sb(f"{pfx}_xsT{i}", [128, KT, RG], BF16) for i in range(2)]
        aT = [P.sb(f"{pfx}_aT{i}", [128, DFF // 128, RG], BF16) for i in range(2)]
        sl = [P.sb(f"{pfx}_sl{i}", [128, RG], F32) for i in range(2)]
        yo32 = [P.sb(f"{pfx}_yo{i}", [128, D // 2], F32) for i in range(2)]
        yo = [t.bitcast(BF16) for t in yo32]
        pT = [P.ps(f"{pfx}_pT{i}", [128, D], BF16) for i in range(1)]
        pg = [P.ps(f"{pfx}_pg{i}", [128, 512], F32) for i in range(2)]
        py = [P.ps(f"{pfx}_py{i}", [128, 512], F32) for i in range(4)]
        xi = 0
        gi = 0
        yi = 0
        ci = 0
        w_in_v = w_in.rearrange("e (k p) n -> e p k n", p=128)
        w_out_v = w_out.rearrange("e (k p) n -> e p k n", p=128)
        for ex in range(NE):
            wb = ex % 2
            kw1, kw2 = f"{pfx}_w1{wb}", f"{pfx}_w2{wb}"
            P.dma("pool", lambda e, ex=ex, wb=wb: e.dma_start(out=w1[wb][:], in_=w_in_v[ex]), key=kw1, writes=[kw1])
            P.dma("pool", lambda e, ex=ex, wb=wb: e.dma_start(out=w2[wb][:], in_=w_out_v[ex]), key=kw2, writes=[kw2])
            for sub in range(CAP // RG):
                cb = ci % 2
                ci += 1
                kxT = f"{pfx}_xsT{cb}"
                for rt in range(RG // 128):
                    xb = xi % 4
                    xi += 1
                    kxr = f"{pfx}_xr{xb}"
                    row0 = ex * CAP + sub * RG + rt * 128
                    P.dma("sp", lambda e, xb=xb, row0=row0: e.dma_start(out=xr[xb][:], in_=xs[row0:row0 + 128, :]),
                          key=kxr, writes=[kxr])

                    def tr(e, xb=xb):
                        last = None
                        for k in range(KT):
                            last = e.transpose(out=pT[0][:, k * 128:(k + 1) * 128], in_=xr[xb][:, k * 128:(k + 1) * 128], identity=C["idb"][:])
                        return last
                    P.op("pe", tr, reads=[kxr] + CK, writes=[f"{pfx}_pT0"])
                    P.op("act", lambda e, cb=cb, rt=rt: e.activation(
                        out=xsT[cb][:, :, rt * 128:(rt + 1) * 128], in_=pT[0][:].rearrange("p (k t) -> p k t", k=KT), func=AF.Copy),
                        reads=[f"{pfx}_pT0"], writes=[(kxT, rt)])
                kaT = f"{pfx}_aT{cb}"
                for j in range(DFF // 128):
                    g = gi % 2
                    gi += 1
                    kpg = f"{pfx}_pg{g}"

                    def mmg(e, wb=wb, cb=cb, j=j, g=g):
                        last = None
                        for half, jj in ((0, j), (1, j + DFF // 128)):
                            for k in range(KT):
                                last = e.matmul(pg[g][:, half * RG:(half + 1) * RG], lhsT=w1[wb][:, k, jj * 128:(jj + 1) * 128],
                                                rhs=xsT[cb][:, k, :], start=(k == 0), stop=(k == KT - 1))
                        return last
                    P.op("pe", mmg, reads=[kw1] + [(kxT, rt) for rt in range(RG // 128)], writes=[kpg])
                    P.op("act", lambda e, g=g: e.activation(out=sl[g][:], in_=pg[g][:, 0:RG], func=AF.Silu),
                         reads=[kpg], writes=[f"{pfx}_sl{g}"])
                    P.op("dve", lambda e, g=g, cb=cb, j=j: e.tensor_tensor(out=aT[cb][:, j, :], in0=sl[g][:], in1=pg[g][:, RG:2 * RG], op=ALU.mult),
                         reads=[f"{pfx}_sl{g}", kpg], writes=[(kaT, j)])
                for rt in range(RG // 128):
                    yb = yi % 2
                    yi += 1
                    kyo = f"{pfx}_yo{yb}"
                    for n in range(D // 512):
                        pb = n
                        kpy = f"{pfx}_py{pb}"

                        def mmy(e, wb=wb, cb=cb, rt=rt, n=n, pb=pb):
                            last = None
                            for k in range(DFF // 128):
                                last = e.matmul(py[pb][:], lhsT=aT[cb][:, k, rt * 128:(rt + 1) * 128], rhs=w2[wb][:, k, n * 512:(n + 1) * 512],
                                                start=(k == 0), stop=(k == DFF // 128 - 1))
                            return last
                        P.op("pe", mmy, reads=[kw2] + [(kaT, j) for j in range(DFF // 128)], writes=[kpy])
                        if n % 2 == 0:
                            P.op("act", lambda e, yb=yb, n=n, pb=pb: e.activation(out=yo[yb][:, n * 512:(n + 1) * 512], in_=py[pb][:], func=AF.Copy),
                                 reads=[kpy], writes=[(kyo, n)])
                        else:
                            P.op("dve", lambda e, yb=yb, n=n, pb=pb: e.tensor_copy(out=yo[yb][:, n * 512:(n + 1) * 512], in_=py[pb][:]),
                                 reads=[kpy], writes=[(kyo, n)])
                    row0 = ex * CAP + sub * RG + rt * 128
                    P.dma("sp", lambda e, yb=yb, row0=row0: e.dma_start(out=ys[row0:row0 + 128, :], in_=yo32[yb][:]),
                          key=kyo + "_st", reads=[(kyo, n) for n in range(D // 512)])
    with P.scope():
        xt3 = [P.sb(f"{pfx}_cx{i}", [128, D], F32) for i in range(2)]
        y1 = [P.sb(f"{pfx}_cy1{i}", [128, D // 2], F32) for i in range(2)]
        y2 = [P.sb(f"{pfx}_cy2{i}", [128, D // 2], F32) for i in range(2)]
        for i in range(2):
            P.op("pool", lambda e, i=i: e.memset(y1[i][:], 0.0), writes=[f"{pfx}_cy1{i}"])
            P.op("pool", lambda e, i=i: e.memset(y2[i][:], 0.0), writes=[f"{pfx}_cy2{i}"])
        for i in range(NTILE):
            s = i % 2
            r0 = i * 128
            kx = f"{pfx}_cx{s}"
            P.dma("sp", lambda e, s=s, r0=r0: e.dma_start(out=xt3[s][:], in_=x[r0:r0 + 128, :]), key=kx, writes=[kx])
            for k2, yy, nm in ((0, y1, "cy1"), (1, y2, "cy2")):
                ky = f"{pfx}_{nm}{s}"
                P.dma("pool", lambda e, s=s, i=i, k2=k2, yy=yy: e.indirect_dma_start(
                    out=yy[s][:, :], out_offset=None, in_=ys[:, :],
                    in_offset=bass.IndirectOffsetOnAxis(ap=dst[:, i, k2:k2 + 1], axis=0), bounds_check=_bc_reg(e, NROWS - 1), oob_is_err=False),
                    key=ky, reads=[(f"{pfx}_dst", i)], writes=[ky])
                P.op("dve", lambda e, s=s, i=i, k2=k2, yy=yy: e.scalar_tensor_tensor(
                    out=xt3[s][:], in0=yy[s].bitcast(BF16), scalar=wts[:, i, k2:k2 + 1], in1=xt3[s][:], op0=ALU.mult, op1=ALU.add),
                    reads=[ky, kx, f"{pfx}_s_wts0", f"{pfx}_s_wts1"], writes=[kx])
            P.dma("sp", lambda e, s=s, r0=r0: e.dma_start(out=xo[r0:r0 + 128, :], in_=xt3[s][:]), key=kx + "_st", reads=[kx])
    return dst, wts


def emit_attn_consts(P):
    c = {}
    idf = P.sb("a_idf", [128, 128], F32)
    E0 = P.sb("a_E0", [128, 128], F32)
    onesb = P.sb("a_onesb", [128, 128], BF16)
    onesf = P.sb("a_onesf", [128, 128], F32)
    P.op("pool", lambda e: e.memset(idf[:], 0.0), writes=["a_idf"])
    P.op("pool", lambda e: e.affine_select(out=idf[:], in_=idf[:], pattern=[[-1, 128]], compare_op=ALU.not_equal,
                                           fill=1.0, base=0, channel_multiplier=1), reads=["a_idf"], writes=["a_idf"])
    P.op("pool", lambda e: e.memset(E0[:], 0.0), writes=["a_E0"])
    P.op("pool", lambda e: e.memset(E0[0:1, :], 1.0), reads=["a_E0"], writes=["a_E0"])
    P.op("pool", lambda e: e.memset(onesf[:], 1.0), writes=["a_onesf"])
    P.op("pool", lambda e: e.tensor_copy(out=onesb[:], in_=onesf[:]), reads=["a_onesf"], writes=["a_onesb"])
    c.update(idf=idf, E0=E0, onesb=onesb, keys=["a_idf", "a_E0", "a_onesb"])
    return c


def emit_cum(P, fT, bfg, cumD, NH, S, pfx="c"):
    CH = min(4096, S)
    with P.scope():
        bt = P.sb(f"{pfx}_b", [NH, 1], F32)
        ones = P.sb(f"{pfx}_ones", [NH, CH], F32)
        P.dma("sp", lambda e: e.dma_start(out=bt[:], in_=bfg.rearrange("(p o) -> p o", o=1)), key=f"{pfx}_b", writes=[f"{pfx}_b"])
        P.op("pool", lambda e: e.memset(ones[:], 1.0), writes=[f"{pfx}_ones"])
        z = [P.sb(f"{pfx}_z{i}", [NH, CH], F32) for i in range(2)]
        t = [P.sb(f"{pfx}_t{i}", [NH, CH], F32) for i in range(2)]
        w = [P.sb(f"{pfx}_w{i}", [NH, CH], F32) for i in range(2)]
        cm = [P.sb(f"{pfx}_cm{i}", [NH, CH], F32) for i in range(2)]
        for c in range(S // CH):
            b = c % 2
            pb = (c - 1) % 2
            K = lambda n: f"{pfx}_{n}{b}"
            P.dma("sp", lambda e, b=b, c=c: e.dma_start(out=z[b][:], in_=fT[:, c * CH:(c + 1) * CH]), key=K("z"), writes=[K("z")])
            P.op("dve", lambda e, b=b: e.tensor_scalar(out=z[b][:], in0=z[b][:], scalar1=bt[:], scalar2=None, op0=ALU.add),
                 reads=[K("z"), f"{pfx}_b"], writes=[K("z")])
            P.op("dve", lambda e, b=b: e.tensor_scalar_mul(out=t[b][:], in0=z[b][:], scalar1=-1.0), reads=[K("z")], writes=[K("t")])
            P.op("dve", lambda e, b=b: e.tensor_tensor(out=w[b][:], in0=z[b][:], in1=t[b][:], op=ALU.max), reads=[K("z"), K("t")], writes=[K("w")])
            P.op("act", lambda e, b=b: e.activation(out=w[b][:], in_=w[b][:], func=AF.Exp, scale=-1.0), reads=[K("w")], writes=[K("w")])
            P.op("act", lambda e, b=b: e.activation(out=w[b][:], in_=w[b][:], func=AF.Ln, bias=1.0), reads=[K("w")], writes=[K("w")])
            P.op("dve", lambda e, b=b: e.tensor_scalar_max(out=t[b][:], in0=t[b][:], scalar1=0.0), reads=[K("t")], writes=[K("t")])
            P.op("dve", lambda e, b=b: e.scalar_tensor_tensor(out=w[b][:], in0=w[b][:], scalar=-1.0, in1=t[b][:], op0=ALU.mult, op1=ALU.subtract),
                 reads=[K("w"), K("t")], writes=[K("w")])
            if c == 0:
                P.op("dve", lambda e, b=b: e.tensor_tensor_scan(out=cm[b][:], data0=ones[:], data1=w[b][:], initial=0.0, op0=ALU.mult, op1=ALU.add),
                     reads=[K("w"), f"{pfx}_ones"], writes=[K("cm")])
            else:
                P.op("dve", lambda e, b=b, pb=pb: e.tensor_tensor_scan(out=cm[b][:], data0=ones[:], data1=w[b][:], initial=cm[pb][:, CH - 1:CH],
                                                                     op0=ALU.mult, op1=ALU.add),
                     reads=[K("w"), f"{pfx}_ones", f"{pfx}_cm{pb}"], writes=[K("cm")])
            P.dma("sp", lambda e, b=b, c=c: e.dma_start(out=cumD[:, c * CH:(c + 1) * CH], in_=cm[b][:]), key=K("cm") + "_st", reads=[K("cm")])


def emit_attn(P, C, QT, KT, Vh, cumD, gateT, OgT, NH, S, pfx="a", og_dt=F32):
    NKT = S // 128
    NQB = S // 512
    scale = HD ** -0.5
    CK = C["keys"]
    with P.scope():
        QTh = P.sb(f"{pfx}_QT", [128, S], BF16)
        KTh = P.sb(f"{pfx}_KT", [128, S], BF16)
        Vt = P.sb(f"{pfx}_V", [128, NKT, HD], BF16)
        cr = P.sb(f"{pfx}_cr", [128, 128], F32)
        ncT = P.sb(f"{pfx}_ncT", [128, 128], F32)
        cq = P.sb(f"{pfx}_cq", [128, 128], F32)
        ncols = sum(4 * (qb + 1) for qb in range(NQB))
        bq = P.sb(f"{pfx}_bq", [128, ncols], F32)
        pT = [P.sb(f"{pfx}_pT{i}", [128, 1024], BF16) for i in range(3)]
        rl = [P.sb(f"{pfx}_rl{i}", [128, 512], F32) for i in range(2)]
        oo = [P.sb(f"{pfx}_oo{i}", [128, 512], F32) for i in range(2)]
        gg = [P.sb(f"{pfx}_gg{i}", [128, 512], F32) for i in range(2)]
        og = [P.sb(f"{pfx}_og{i}", [128, 512], og_dt) for i in range(2)]
        sT = [P.ps(f"{pfx}_sT{i}", [128, 1024], F32) for i in range(3)]
        OT = P.ps(f"{pfx}_OT", [128, 512], F32)
        LT = P.ps(f"{pfx}_LT", [128, 512], F32)
        kOT, kLT = f"{pfx}_OT", f"{pfx}_LT"
        ti = 0
        qi = 0
        for h in range(NH):
            KQ, KK, KV = f"{pfx}_QT", f"{pfx}_KT", f"{pfx}_V"
            P.dma("sp", lambda e, h=h: e.dma_start(out=QTh[:], in_=QT[h * 128:(h + 1) * 128, :]), key=KQ, writes=[KQ])
            P.dma("sp", lambda e, h=h: e.dma_start(out=KTh[:], in_=KT[h * 128:(h + 1) * 128, :]), key=KK, writes=[KK])
            P.dma("sp", lambda e, h=h: e.dma_start(out=Vt[:], in_=Vh[h].rearrange("(kt p) d -> p kt d", p=128)), key=KV, writes=[KV])
            if NKT < 128:
                P.op("dve", lambda e: e.memset(cr[:], 0.0), writes=[f"{pfx}_cr"])
            P.dma("sp", lambda e, h=h: e.dma_start(out=cr[0:NKT, :], in_=cumD[h].rearrange("(kt p) -> kt p", p=128)),
                  key=f"{pfx}_cr", writes=[f"{pfx}_cr"], reads=[f"{pfx}_cr"])
            P.op("pe", lambda e: e.transpose(out=sT[0][:, 0:128], in_=cr[:], identity=C["idf"][:]), reads=[f"{pfx}_cr"] + CK, writes=[f"{pfx}_sT0"])
            P.op("act", lambda e: e.activation(out=ncT[:], in_=sT[0][:, 0:128], func=AF.Copy, scale=-1.0), reads=[f"{pfx}_sT0"], writes=[f"{pfx}_ncT"])
            P.op("pe", lambda e: e.matmul(sT[1][:, 0:128], lhsT=C["E0"][:], rhs=ncT[:], start=True, stop=True),
                 reads=[f"{pfx}_ncT"] + CK, writes=[f"{pfx}_sT1"])
            P.op("act", lambda e: e.activation(out=cq[:], in_=sT[1][:, 0:128], func=AF.Copy, scale=-1.0), reads=[f"{pfx}_sT1"], writes=[f"{pfx}_cq"])
            col = 0
            cols = []
            for qb in range(NQB):
                nkt = 4 * (qb + 1)
                P.op("dve", lambda e, qb=qb, nkt=nkt, col=col: e.tensor_scalar(
                    out=bq[:, col:col + nkt], in0=ncT[:, 0:nkt], scalar1=cq[:, 4 * qb:4 * qb + 1], scalar2=None, op0=ALU.add),
                    reads=[f"{pfx}_ncT", f"{pfx}_cq"], writes=[(f"{pfx}_bq", qb)])
                cols.append(col)
                col += nkt
            for qb in range(NQB):
                nkt = 4 * (qb + 1)
                a = qi % 2
                qi += 1
                kgg = f"{pfx}_gg{a}"
                P.dma("sp", lambda e, h=h, qb=qb, a=a: e.dma_start(out=gg[a][:], in_=gateT[h * 128:(h + 1) * 128, qb * 512:(qb + 1) * 512]),
                      key=kgg, writes=[kgg])
                for g2 in range(nkt // 2):
                    r = ti % 3
                    ti += 1
                    kt0 = 2 * g2
                    ksT, kpT = f"{pfx}_sT{r}", f"{pfx}_pT{r}"

                    def mmqk(e, r=r, kt0=kt0, qb=qb):
                        e.matmul(sT[r][:, 0:512], lhsT=KTh[:, kt0 * 128:(kt0 + 1) * 128], rhs=QTh[:, qb * 512:(qb + 1) * 512], start=True, stop=True)
                        return e.matmul(sT[r][:, 512:1024], lhsT=KTh[:, (kt0 + 1) * 128:(kt0 + 2) * 128], rhs=QTh[:, qb * 512:(qb + 1) * 512],
                                        start=True, stop=True)
                    P.op("pe", mmqk, reads=[KQ, KK], writes=[ksT])
                    bc = cols[qb] + kt0

                    def ex2(e, r=r, bc=bc):
                        e.activation(out=pT[r][:, 0:512], in_=sT[r][:, 0:512], func=AF.Exp, scale=scale, bias=bq[:, bc:bc + 1])
                        return e.activation(out=pT[r][:, 512:1024], in_=sT[r][:, 512:1024], func=AF.Exp, scale=scale, bias=bq[:, bc + 1:bc + 2])
                    P.op("act", ex2, reads=[ksT, (f"{pfx}_bq", qb)], writes=[kpT])
                    if kt0 >= 4 * qb:
                        def msk(e, r=r, kt0=kt0, qb=qb):
                            e.affine_select(out=pT[r][:, 0:512], in_=pT[r][:, 0:512], pattern=[[1, 512]], compare_op=ALU.is_ge,
                                            fill=0.0, base=qb * 512 - kt0 * 128, channel_multiplier=-1)
                            return e.affine_select(out=pT[r][:, 512:1024], in_=pT[r][:, 512:1024], pattern=[[1, 512]], compare_op=ALU.is_ge,
                                                   fill=0.0, base=qb * 512 - (kt0 + 1) * 128, channel_multiplier=-1)
                        P.op("pool", msk, reads=[kpT], writes=[kpT])

                    def mm2(e, r=r, kt0=kt0, nkt=nkt):
                        last = None
                        for t in range(2):
                            kt = kt0 + t
                            e.matmul(OT[:], lhsT=Vt[:, kt, :], rhs=pT[r][:, t * 512:(t + 1) * 512], start=(kt == 0), stop=(kt == nkt - 1))
                            last = e.matmul(LT[:], lhsT=C["onesb"][:], rhs=pT[r][:, t * 512:(t + 1) * 512], start=(kt == 0), stop=(kt == nkt - 1))
                        return last
                    P.op("pe", mm2, reads=[kpT, KV] + CK, writes=[kOT, kLT])
                P.op("dve", lambda e, a=a: e.reciprocal(out=rl[a][:], in_=LT[:]), reads=[kLT], writes=[f"{pfx}_rl{a}"])
                P.op("dve", lambda e, a=a: e.tensor_tensor(out=oo[a][:], in0=OT[:], in1=rl[a][:], op=ALU.mult),
                     reads=[kOT, f"{pfx}_rl{a}"], writes=[f"{pfx}_oo{a}"])
                P.op("act", lambda e, a=a: e.activation(out=gg[a][:], in_=gg[a][:], func=AF.Sigmoid), reads=[kgg], writes=[kgg])
                P.op("pool", lambda e, a=a: e.tensor_tensor(out=og[a][:], in0=oo[a][:], in1=gg[a][:], op=ALU.mult),
                     reads=[f"{pfx}_oo{a}", kgg], writes=[f"{pfx}_og{a}"])
                P.dma("sp", lambda e, h=h, qb=qb, a=a: e.dma_start(out=OgT[h * 128:(h + 1) * 128, qb * 512:(qb + 1) * 512], in_=og[a][:]),
                      key=f"{pfx}_og{a}_st", reads=[f"{pfx}_og{a}"])


def emit_linear2(P, C, T, D, src, outs, pfx="l"):
    CK = C["keys"]
    KT = D // 128
    TG = 1024
    NT = TG // 128
    NB = 512
    with P.scope():
        hT = P.sb(f"{pfx}_hT", [128, KT, TG], BF16)
        Wb = [P.sb(f"{pfx}_Wb{i}", [128, KT, NB], BF16) for i in range(2)]
        otf = [P.sb(f"{pfx}_otf{i}", [128, NB], F32) for i in range(3)]
        otb = [P.sb(f"{pfx}_otb{i}", [128, NB], BF16) for i in range(3)]
        rt = [P.sb(f"{pfx}_rt{i}", [128, NB], F32) for i in range(3)]
        acc = [P.ps(f"{pfx}_acc{i}", [128, NB], F32) for i in range(4)]
        if src["kind"] == "tok":
            xt = [P.sb(f"{pfx}_xt{i}", [128, D], F32) for i in range(2)]
            junk = P.sb(f"{pfx}_junk", [128, D], BF16)
            hb = [P.sb(f"{pfx}_hb{i}", [128, D], BF16) for i in range(2)]
            ss = [P.sb(f"{pfx}_ss{i}", [128, 1], F32) for i in range(2)]
            sd = [P.sb(f"{pfx}_sd{i}", [128, 1], F32) for i in range(2)]
            rs = [P.sb(f"{pfx}_rs{i}", [128, 1], F32) for i in range(2)]
            gT = P.sb(f"{pfx}_gT", [128, KT], F32)
            pT = [P.ps(f"{pfx}_pT{i}", [128, D], BF16) for i in range(2)]
            g2 = src["gain"].rearrange("(k p o) -> k p o", p=128, o=1)
            for k in range(KT):
                P.dma("sp", lambda e, k=k: e.dma_start(out=gT[:, k:k + 1], in_=g2[k]), key=f"{pfx}_gT", writes=[f"{pfx}_gT"])
        ti_glob = 0
        wi = 0
        oi = 0
        for g in range(T // TG):
            if src["kind"] == "tok":
                x = src["x"]
                for i in range(NT):
                    s = ti_glob % 2
                    ti_glob += 1
                    r0 = g * TG + i * 128
                    kx, khb, kp = f"{pfx}_xt{s}", f"{pfx}_hb{s}", f"{pfx}_pT{s}"
                    P.dma("sp", lambda e, s=s, r0=r0: e.dma_start(out=xt[s][:], in_=x[r0:r0 + 128, :]), key=kx, writes=[kx])
                    P.op("act", lambda e, s=s: e.activation(out=junk[:], in_=xt[s][:], func=AF.Square, accum_out=ss[s][:]),
                         reads=[kx], writes=[f"{pfx}_junk", f"{pfx}_ss{s}"])
                    P.op("act", lambda e, s=s: e.activation(out=sd[s][:], in_=ss[s][:], func=AF.Sqrt, scale=1.0 / D, bias=C["eps"][:]),
                         reads=[f"{pfx}_ss{s}"] + CK, writes=[f"{pfx}_sd{s}"])
                    P.op("dve", lambda e, s=s: e.reciprocal(out=rs[s][:], in_=sd[s][:]), reads=[f"{pfx}_sd{s}"], writes=[f"{pfx}_rs{s}"])
                    P.op("act", lambda e, s=s: e.activation(out=hb[s][:], in_=xt[s][:], func=AF.Copy, scale=rs[s][:]),
                         reads=[kx, f"{pfx}_rs{s}"], writes=[khb])

                    def tr(e, s=s):
                        last = None
                        for k in range(KT):
                            last = e.transpose(out=pT[s][:, k * 128:(k + 1) * 128], in_=hb[s][:, k * 128:(k + 1) * 128], identity=C["idb"][:])
                        return last
                    P.op("pe", tr, reads=[khb] + CK, writes=[kp])
                    P.op("dve", lambda e, s=s, i=i: e.tensor_tensor(
                        out=hT[:, :, i * 128:(i + 1) * 128], in0=pT[s][:].rearrange("p (k t) -> p k t", k=KT),
                        in1=gT[:].unsqueeze(2).to_broadcast([128, KT, 128]), op=ALU.mult),
                        reads=[kp, f"{pfx}_gT"], writes=[(f"{pfx}_hT", i)])
            else:
                xTv = src["xT"].rearrange("(k p) t -> p k t", p=128)
                P.dma("pool", lambda e, g=g: e.dma_start(out=hT[:], in_=xTv[:, :, g * TG:(g + 1) * TG]), key=f"{pfx}_hTd",
                      writes=[(f"{pfx}_hT", i) for i in range(NT)])
            for o in outs:
                n = o["n"]
                Wv = o["W"].rearrange("(k p) n -> p k n", p=128)
                for blk in range((n + NB - 1) // NB):
                    c0 = blk * NB
                    nb = min(NB, n - c0)
                    ws = wi % 2
                    wi += 1
                    kw = f"{pfx}_Wb{ws}"
                    P.dma("pool", lambda e, ws=ws, c0=c0, nb=nb, Wv=Wv: e.dma_start(out=Wb[ws][:, :, 0:nb], in_=Wv[:, :, c0:c0 + nb]),
                          key=kw, writes=[kw])
                    ot = otb if o["dt"] == BF16 else otf
                    otn = "otb" if o["dt"] == BF16 else "otf"
                    if o["mode"] == "tok":
                        for i in range(NT):
                            a = oi % 4
                            q = oi % 3
                            oi += 1
                            ka, ko = f"{pfx}_acc{a}", f"{pfx}_{otn}{q}"
                            r0 = g * TG + i * 128

                            def mm(e, ws=ws, i=i, a=a, nb=nb):
                                last = None
                                for k in range(KT):
                                    last = e.matmul(acc[a][:, 0:nb], lhsT=hT[:, k, i * 128:(i + 1) * 128], rhs=Wb[ws][:, k, 0:nb],
                                                    start=(k == 0), stop=(k == KT - 1))
                                return last
                            P.op("pe", mm, reads=[(f"{pfx}_hT", i), kw], writes=[ka])
                            if o.get("resid") is not None:
                                kr = f"{pfx}_rt{q}"
                                P.dma("sp", lambda e, q=q, r0=r0, c0=c0, nb=nb, o=o: e.dma_start(
                                    out=rt[q][:, 0:nb], in_=o["resid"][r0:r0 + 128, c0:c0 + nb]), key=kr, writes=[kr])
                                P.op("dve", lambda e, a=a, q=q, nb=nb, ot=ot: e.tensor_tensor(out=ot[q][:, 0:nb], in0=acc[a][:, 0:nb], in1=rt[q][:, 0:nb], op=ALU.add),
                                     reads=[ka, kr], writes=[ko])
                            elif oi % 2 == 0:
                                P.op("act", lambda e, a=a, q=q, nb=nb, ot=ot: e.activation(out=ot[q][:, 0:nb], in_=acc[a][:, 0:nb], func=AF.Copy),
                                     reads=[ka], writes=[ko])
                            else:
                                P.op("dve", lambda e, a=a, q=q, nb=nb, ot=ot: e.tensor_copy(out=ot[q][:, 0:nb], in_=acc[a][:, 0:nb]),
                                     reads=[ka], writes=[ko])
                            if o.get("heads"):
                                for jj in range(nb // 128):
                                    hh = (c0 + jj * 128) // 128
                                    P.dma("sp", lambda e, q=q, r0=r0, jj=jj, hh=hh, o=o, ot=ot: e.dma_start(
                                        out=o["dst"][hh, r0:r0 + 128, :], in_=ot[q][:, jj * 128:(jj + 1) * 128]), key=ko + "_st", reads=[ko])
                            else:
                                P.dma("sp", lambda e, q=q, r0=r0, c0=c0, nb=nb, o=o, ot=ot: e.dma_start(
                                    out=o["dst"][r0:r0 + 128, c0:c0 + nb], in_=ot[q][:, 0:nb]), key=ko + "_st", reads=[ko])
                    else:
                        for j in range((nb + 127) // 128):
                            mc = min(128, nb - j * 128)
                            for qq in range(TG // 512):
                                a = oi % 4
                                q = oi % 3
                                oi += 1
                                ka, ko = f"{pfx}_acc{a}", f"{pfx}_{otn}{q}"
                                t0 = g * TG + qq * 512

                                def mmT(e, ws=ws, j=j, mc=mc, qq=qq, a=a):
                                    last = None
                                    for k in range(KT):
                                        last = e.matmul(acc[a][0:mc, :], lhsT=Wb[ws][:, k, j * 128:j * 128 + mc], rhs=hT[:, k, qq * 512:(qq + 1) * 512],
                                                        start=(k == 0), stop=(k == KT - 1))
                                    return last
                                P.op("pe", mmT, reads=[(f"{pfx}_hT", 4 * qq + t) for t in range(4)] + [kw], writes=[ka])
                                if oi % 2 == 0:
                                    P.op("act", lambda e, a=a, q=q, mc=mc, ot=ot: e.activation(out=ot[q][0:mc, :], in_=acc[a][0:mc, :], func=AF.Copy),
                                         reads=[ka], writes=[ko])
                                else:
                                    P.op("dve", lambda e, a=a, q=q, mc=mc, ot=ot: e.tensor_copy(out=ot[q][0:mc, :], in_=acc[a][0:mc, :]),
                                         reads=[ka], writes=[ko])
                                P.dma("sp", lambda e, q=q, mc=mc, c0=c0, j=j, t0=t0, o=o, ot=ot: e.dma_start(
                                    out=o["dst"][c0 + j * 128:c0 + j * 128 + mc, t0:t0 + 512], in_=ot[q][0:mc, :]), key=ko + "_st", reads=[ko])


def emit_final_norm(P, C, x, gain, out, T, D, pfx="f"):
    CK = C["keys"]
    with P.scope():
        gbc = P.sb(f"{pfx}_gbc", [128, D], F32)
        P.dma("sp", lambda e: e.dma_start(out=gbc[:], in_=gain.partition_broadcast(128)), key=f"{pfx}_g", writes=[f"{pfx}_g"])
        xt = [P.sb(f"{pfx}_xt{i}", [128, D], F32) for i in range(2)]
        yt = [P.sb(f"{pfx}_yt{i}", [128, D], F32) for i in range(2)]
        junk = P.sb(f"{pfx}_junk", [128, D], BF16)
        ss = [P.sb(f"{pfx}_ss{i}", [128, 1], F32) for i in range(2)]
        sd = [P.sb(f"{pfx}_sd{i}", [128, 1], F32) for i in range(2)]
        rs = [P.sb(f"{pfx}_rs{i}", [128, 1], F32) for i in range(2)]
        for i in range(T // 128):
            s = i % 2
            r0 = i * 128
            kx = f"{pfx}_xt{s}"
            P.dma("sp", lambda e, s=s, r0=r0: e.dma_start(out=xt[s][:], in_=x[r0:r0 + 128, :]), key=kx, writes=[kx])
            P.op("act", lambda e, s=s: e.activation(out=junk[:], in_=xt[s][:], func=AF.Square, accum_out=ss[s][:]),
                 reads=[kx], writes=[f"{pfx}_junk", f"{pfx}_ss{s}"])
            P.op("act", lambda e, s=s: e.activation(out=sd[s][:], in_=ss[s][:], func=AF.Sqrt, scale=1.0 / D, bias=C["eps"][:]),
                 reads=[f"{pfx}_ss{s}"] + CK, writes=[f"{pfx}_sd{s}"])
            P.op("dve", lambda e, s=s: e.reciprocal(out=rs[s][:], in_=sd[s][:]), reads=[f"{pfx}_sd{s}"], writes=[f"{pfx}_rs{s}"])
            P.op("dve", lambda e, s=s: e.scalar_tensor_tensor(out=yt[s][:], in0=xt[s][:], scalar=rs[s][:], in1=gbc[:], op0=ALU.mult, op1=ALU.mult),
                 reads=[kx, f"{pfx}_rs{s}", f"{pfx}_g"], writes=[f"{pfx}_yt{s}"])
            P.dma("sp", lambda e, s=s, r0=r0: e.dma_start(out=out[r0:r0 + 128, :], in_=yt[s][:]), key=f"{pfx}_yt{s}_st", reads=[f"{pfx}_yt{s}"])


SEQ = 16384
DM = 2048
NHEAD = 16

IN_SHAPES = dict(
    x=[SEQ, DM], a_norm=[1, DM], a_w_in=[1, DM, 2 * DM], a_conv_w=[1, 4, DM], a_conv_b=[1, DM], a_w_rec=[1, 8, 256, 256],
    a_b_rec=[1, DM], a_w_inp=[1, 8, 256, 256], a_b_inp=[1, DM], a_lambda=[1, DM], a_w_out=[1, DM, DM], kv_norm=[DM],
    kv_w=[DM, 2 * DM + NHEAD], kv_b_forget=[NHEAD], b_norm=[1, DM], b_w_qg=[1, DM, 2 * DM], b_w_o=[1, DM, DM], m_norm=[2, DM],
    m_w_group=[2, DM, NG], m_b_group=[2, NG], m_w_router=[2, DM, NE], m_b_router=[2, NE], m_w_in=[2, NE, DM, 2 * DFF],
    m_w_out=[2, NE, DFF, DM], final_norm=[DM])


def build_program(S=SEQ, stages=None):
    nc = bass.Bass("TRN2", target_bir_lowering=False)
    I = {}
    for k, shp in IN_SHAPES.items():
        shp = [S, DM] if k == "x" else shp
        I[k] = nc.dram_tensor(k, shp, F32, kind="ExternalInput").ap()
    out = nc.dram_tensor("out", [S, DM], F32, kind="ExternalOutput").ap()
    sc = lambda n, s, dt=F32: nc.dram_tensor(n, s, dt, kind="Internal").ap()
    projT = sc("projT", [2 * DM, S]); outT = sc("outT", [DM, S])
    x1 = sc("x1", [S, DM]); x2 = sc("x2", [S, DM]); x3 = sc("x3", [S, DM]); x4 = sc("x4", [S, DM])
    xs = sc("xs", [NE * CAP, DM], BF16); ys = sc("ys", [NE * CAP, DM // 2])
    KTd = sc("KTd", [DM, S], BF16); QTd = sc("QTd", [DM, S], BF16); Vh = sc("Vh", [NHEAD, S, HD], BF16)
    fT = sc("fT", [NHEAD, S]); cumD = sc("cumD", [NHEAD, S]); gateT = sc("gateT", [DM, S]); OgT = sc("OgT", [DM, S])
    with ExitStack() as es:
        P = Prog(nc, es)
        C = emit_moe_consts(P)
        onesb = P.sb("c_onesb", [128, 128], BF16)
        P.op("pool", lambda e: e.tensor_copy(out=onesb[:], in_=C["ones"][:]), reads=["c_ones"], writes=["c_onesb"])
        C["onesb"] = onesb
        C["keys"] = C["keys"] + ["c_onesb"]
        ONE_AP[0] = C["ones"][:, 0:1]
        emit_linear2(P, C, S, DM, dict(kind="tok", x=I["x"], gain=I["a_norm"][0]),
                     [dict(W=I["a_w_in"][0], n=2 * DM, mode="T", dst=projT, dt=F32)])
        for n in range(8):
            with P.scope():
                fs = slice(n * 256, (n + 1) * 256)
                emit_rglru(P, projT[n * 256:(n + 1) * 256, :], projT[DM + n * 256:DM + (n + 1) * 256, :], outT[n * 256:(n + 1) * 256, :],
                           I["a_conv_w"][0][:, fs], I["a_conv_b"][0][fs], I["a_w_rec"][0][n], I["a_b_rec"][0][fs], I["a_w_inp"][0][n],
                           I["a_b_inp"][0][fs], I["a_lambda"][0][fs], 1, S)
        emit_linear2(P, C, S, DM, dict(kind="T", xT=outT), [dict(W=I["a_w_out"][0], n=DM, mode="tok", dst=x1, dt=F32, resid=I["x"])])
        with P.scope():
            emit_moe(P, C, x1, x2, I["m_norm"][0], I["m_w_group"][0], I["m_b_group"][0], I["m_w_router"][0], I["m_b_router"][0],
                     I["m_w_in"][0], I["m_w_out"][0], xs, ys, S)
        emit_linear2(P, C, S, DM, dict(kind="tok", x=x2, gain=I["kv_norm"]),
                     [dict(W=I["kv_w"][:, 0:DM], n=DM, mode="T", dst=KTd, dt=BF16),
                      dict(W=I["kv_w"][:, DM:2 * DM], n=DM, mode="tok", dst=Vh, dt=BF16, heads=True),
                      dict(W=I["kv_w"][:, 2 * DM:2 * DM + NHEAD], n=NHEAD, mode="T", dst=fT, dt=F32)])
        emit_linear2(P, C, S, DM, dict(kind="tok", x=x2, gain=I["b_norm"][0]),
                     [dict(W=I["b_w_qg"][0][:, 0:DM], n=DM, mode="T", dst=QTd, dt=BF16),
                      dict(W=I["b_w_qg"][0][:, DM:2 * DM], n=DM, mode="T", dst=gateT, dt=F32)])
        CA = dict(idf=C["idf"], E0=C["E0"], onesb=C["onesb"], keys=C["keys"])
        emit_cum(P, fT, I["kv_b_forget"], cumD, NHEAD, S)
        emit_attn(P, CA, QTd, KTd, Vh, cumD, gateT, OgT, NHEAD, S)
        emit_linear2(P, C, S, DM, dict(kind="T", xT=OgT), [dict(W=I["b_w_o"][0], n=DM, mode="tok", dst=x3, dt=F32, resid=x2)])
        with P.scope():
            emit_moe(P, C, x3, x4, I["m_norm"][1], I["m_w_group"][1], I["m_b_group"][1], I["m_w_router"][1], I["m_b_router"][1],
                     I["m_w_in"][1], I["m_w_out"][1], xs, ys, S)
        emit_final_norm(P, C, x4, I["final_norm"], out, S, DM)
        P.wait_all("sp")
        P.n_instr = {e: len(v) for e, v in P.streams.items()}
        P.run()
    return nc, P


def kernel(**inputs):
    nc, _ = build_program()
    names = list(IN_SHAPES.keys())
    x = np.ascontiguousarray(inputs["x"], dtype=np.float32)
    in_maps = []
    for b in range(2):
        m = {k: np.ascontiguousarray(inputs[k], dtype=np.float32) for k in names if k != "x"}
        m["x"] = x[b]
        in_maps.append(m)
    res = run_bass_kernel_spmd(nc, in_maps, core_ids=[0, 1])
    return np.stack([res.results[b]["out"] for b in range(2)], axis=0).astype(np.float32)
```
